# Optimizing a Trainium2 kernel written in Bass

```python
import jax, jax.numpy as jnp
from jax import lax
import numpy as np

D_MODEL = 2048
BATCH = 4
SEQ = 4096
DEPTH = 1

N_META = 16
CHUNK = 128
RET_HEADS = 4
RET_QK = 128
RET_V = 256
RET_ROT_BASE = 10000.0
DIFF_HEADS = 4
DIFF_QK = 128
DIFF_V = 256
ROT_DIMS = DIFF_QK // 4
ROPE_THETA = 500000.0
N_GROUPS = 8
EXPERTS_PER_GROUP = 8
N_EXPERTS = N_GROUPS * EXPERTS_PER_GROUP
TOP_K = 2
EXPERT_FF = 512
EPS = 1e-6
NEG_INF = -1e30

RET_WIDTH = RET_HEADS * RET_V
DIFF_WIDTH = DIFF_HEADS * DIFF_V
MIX_WIDTH = RET_WIDTH + DIFF_WIDTH
IN_SPLITS = [RET_HEADS * RET_QK, RET_HEADS * RET_QK, RET_WIDTH, RET_WIDTH,
             DIFF_HEADS * 2 * DIFF_QK, DIFF_HEADS * 2 * DIFF_QK, DIFF_WIDTH]
IN_WIDTH = int(sum(IN_SPLITS))
IN_OFFSETS = [int(o) for o in np.cumsum(IN_SPLITS)[:-1]]

kernel_name = "hymba_retnet_diffattn_hiermoe"


def _rms(x):
    xf = x.astype(jnp.float32)
    return (xf * lax.rsqrt(jnp.mean(xf * xf, -1, keepdims=True) + EPS)).astype(x.dtype)


def rmsnorm(x, g):
    xf = x.astype(jnp.float32)
    y = xf * lax.rsqrt(jnp.mean(xf * xf, -1, keepdims=True) + EPS)
    return (y * g.astype(jnp.float32)).astype(x.dtype)


def rope_tables(pos, inv_freq, dtype):
    ang = pos[:, None] * inv_freq[None, :]
    return jnp.cos(ang).astype(dtype), jnp.sin(ang).astype(dtype)


def apply_rope(x, cos, sin):
    x1, x2 = jnp.split(x, 2, axis=-1)
    return jnp.concatenate([x1 * cos - x2 * sin, x1 * sin + x2 * cos], axis=-1)


def apply_partial_rope(x, cos, sin):
    return jnp.concatenate([apply_rope(x[..., :ROT_DIMS], cos, sin), x[..., ROT_DIMS:]], axis=-1)


def to_chunks(t, n_chunks):
    b, h, _, d = t.shape
    return t.reshape(b, h, n_chunks, CHUNK, d).transpose(2, 0, 1, 3, 4)


def from_chunks(t):
    n, b, h, c, d = t.shape
    return t.transpose(1, 0, 3, 2, 4).reshape(b, n * c, h * d)


def retention(q, k, v, gate, pos, valid):
    dt = q.dtype
    n_chunks = q.shape[2] // CHUNK
    inv = 1.0 / (RET_ROT_BASE ** jnp.linspace(0.0, 1.0, RET_QK // 2, dtype=jnp.float32))
    cos, sin = rope_tables(pos, inv, dt)
    vm = valid.astype(dt)[None, None, :, None]
    q = apply_rope(q, cos, sin)
    k = apply_rope(k, cos, sin) * (RET_QK ** -0.5) * vm
    v = v * vm
    log_g = jnp.log(1.0 - 2.0 ** (-5.0 - jnp.arange(RET_HEADS, dtype=jnp.float32)))
    idx = jnp.arange(CHUNK, dtype=jnp.float32)
    dist = idx[:, None] - idx[None, :]
    decay_mask = jnp.where(dist >= 0, jnp.exp(jnp.maximum(dist, 0.0)[None] * log_g[:, None, None]), 0.0).astype(dt)
    xi = jnp.exp((idx + 1.0)[None, :] * log_g[:, None]).astype(dt)
    zeta = jnp.exp((CHUNK - 1.0 - idx)[None, :] * log_g[:, None]).astype(dt)
    chunk_decay = jnp.exp(CHUNK * log_g).astype(dt)
    qc, kc, vc = to_chunks(q, n_chunks), to_chunks(k, n_chunks), to_chunks(v, n_chunks)
    scores = jnp.einsum('nbhcd,nbhsd->nbhcs', qc, kc) * decay_mask[None, None]
    inner = jnp.einsum('nbhcs,nbhse->nbhce', scores, vc)
    kv = jnp.einsum('nbhcd,nbhce->nbhde', kc * zeta[None, None, :, :, None], vc)

    def step(state, inp):
        q_i, kv_i = inp
        o = jnp.einsum('bhcd,bhde->bhce', q_i, state)
        return state * chunk_decay[None, :, None, None] + kv_i, o

    b = q.shape[0]
    state0 = jnp.zeros((b, RET_HEADS, RET_QK, RET_V), dt)
    _, cross = lax.scan(step, state0, (qc, kv))
    out = inner + cross * xi[None, None, :, :, None]
    out = from_chunks(_rms(out))
    return jax.nn.silu(gate) * out


def diff_attention(q, k, v, pos, valid, lq1, lk1, lq2, lk2, subln_g, lambda_init):
    dt = q.dtype
    L = q.shape[3]
    n_blocks = L // CHUNK
    inv = ROPE_THETA ** (-jnp.arange(0, ROT_DIMS, 2, dtype=jnp.float32) / ROT_DIMS)
    cos, sin = rope_tables(pos, inv, dt)
    q = apply_partial_rope(q, cos, sin) * (DIFF_QK ** -0.5)
    k = apply_partial_rope(k, cos, sin)
    lam = (jnp.exp(jnp.sum(lq1.astype(jnp.float32) * lk1.astype(jnp.float32)))
           - jnp.exp(jnp.sum(lq2.astype(jnp.float32) * lk2.astype(jnp.float32))) + lambda_init)
    b = q.shape[0]
    qb = q.reshape(b, 2, DIFF_HEADS, n_blocks, CHUNK, DIFF_QK).transpose(3, 0, 1, 2, 4, 5)
    kpos = jnp.arange(L)

    def block(args):
        q_blk, i = args
        s = jnp.einsum('bjhcd,bjhsd->bjhcs', q_blk, k).astype(jnp.float32)
        qpos = i * CHUNK + jnp.arange(CHUNK)
        allowed = (kpos[None, :] <= qpos[:, None]) & valid[None, :]
        p = jax.nn.softmax(jnp.where(allowed, s, NEG_INF), axis=-1)
        a = (p[:, 0] - lam * p[:, 1]).astype(v.dtype)
        return jnp.einsum('bhcs,bhse->bhce', a, v)

    out = lax.map(block, (qb, jnp.arange(n_blocks)))
    out = rmsnorm(out, subln_g) * (1.0 - lambda_init)
    return from_chunks(out)


def hier_moe(u, w_group_router, w_expert_router, w_gate, w_up, w_down):
    t = u.shape[0]
    g_logits = jnp.einsum('td,dg->tg', u, w_group_router).astype(jnp.float32)
    g_prob = jax.nn.softmax(g_logits, axis=-1)
    g_star = jnp.argmax(g_prob, axis=-1)
    g_gate = jnp.take_along_axis(g_prob, g_star[:, None], axis=-1)[:, 0]
    e_logits = jnp.einsum('td,de->te', u, w_expert_router).astype(jnp.float32)
    e_logits = e_logits.reshape(t, N_GROUPS, EXPERTS_PER_GROUP)
    e_sel = jnp.take_along_axis(e_logits, g_star[:, None, None], axis=1)[:, 0]
    e_prob = jax.nn.softmax(e_sel, axis=-1)
    top_p, top_i = lax.top_k(e_prob, TOP_K)
    top_p = top_p / jnp.sum(top_p, axis=-1, keepdims=True)
    weights = (g_gate[:, None] * top_p).astype(u.dtype)
    expert_id = (g_star[:, None] * EXPERTS_PER_GROUP + top_i).reshape(-1)
    order = jnp.argsort(expert_id)
    tok = order // TOP_K
    xs = u[tok]
    sizes = jnp.bincount(expert_id, length=N_EXPERTS).astype(jnp.int32)
    hg = lax.ragged_dot(xs, w_gate, sizes)
    hu = lax.ragged_dot(xs, w_up, sizes)
    y = lax.ragged_dot(jax.nn.silu(hg) * hu, w_down, sizes)
    y = y * weights.reshape(-1)[order][:, None]
    return jnp.zeros_like(u).at[tok].add(y)


def setup_inputs(seed: int = 0) -> dict:
    key = jax.random.key(seed)
    ks = jax.random.split(key, 17)
    f32 = jnp.float32
    nrm = lambda k, shape, scale: (jax.random.normal(k, shape, f32) * scale).astype(f32)
    gain = lambda k, shape: (1.0 + 0.01 * jax.random.normal(k, shape, f32)).astype(f32)
    return {
        "x": nrm(ks[0], (BATCH, SEQ, D_MODEL), 1.0),
        "meta_tokens": nrm(ks[1], (N_META, D_MODEL), 1.0),
        "attn_norm_g": gain(ks[2], (DEPTH, D_MODEL)),
        "w_in": nrm(ks[3], (DEPTH, D_MODEL, IN_WIDTH), D_MODEL ** -0.5),
        "w_out": nrm(ks[4], (DEPTH, MIX_WIDTH, D_MODEL), MIX_WIDTH ** -0.5),
        "lambda_q1": nrm(ks[5], (DEPTH, DIFF_QK), 0.1),
        "lambda_k1": nrm(ks[6], (DEPTH, DIFF_QK), 0.1),
        "lambda_q2": nrm(ks[7], (DEPTH, DIFF_QK), 0.1),
        "lambda_k2": nrm(ks[8], (DEPTH, DIFF_QK), 0.1),
        "diff_subln_g": gain(ks[9], (DEPTH, DIFF_V)),
        "ffn_norm_g": gain(ks[10], (DEPTH, D_MODEL)),
        "w_group_router": nrm(ks[11], (DEPTH, D_MODEL, N_GROUPS), D_MODEL ** -0.5),
        "w_expert_router": nrm(ks[12], (DEPTH, D_MODEL, N_EXPERTS), D_MODEL ** -0.5),
        "w_gate": nrm(ks[13], (DEPTH, N_EXPERTS, D_MODEL, EXPERT_FF), D_MODEL ** -0.5),
        "w_up": nrm(ks[14], (DEPTH, N_EXPERTS, D_MODEL, EXPERT_FF), D_MODEL ** -0.5),
        "w_down": nrm(ks[15], (DEPTH, N_EXPERTS, EXPERT_FF, D_MODEL), EXPERT_FF ** -0.5),
        "final_norm_g": gain(ks[16], (D_MODEL,)),
    }


def reference(x, meta_tokens, attn_norm_g, w_in, w_out, lambda_q1, lambda_k1, lambda_q2, lambda_k2,
              diff_subln_g, ffn_norm_g, w_group_router, w_expert_router, w_gate, w_up, w_down,
              final_norm_g):
    b, s, d = x.shape
    dt = x.dtype
    n_pad = CHUNK - N_META
    L = CHUNK + s
    h = jnp.concatenate([jnp.zeros((b, n_pad, d), dt),
                         jnp.broadcast_to(meta_tokens.astype(dt)[None], (b, N_META, d)), x], axis=1)
    pos_i = jnp.arange(L) - n_pad
    valid = pos_i >= 0
    pos = pos_i.astype(jnp.float32)
    row_mask = valid.astype(dt)[None, :, None]

    for l in range(DEPTH):
        lambda_init = 0.8 - 0.6 * float(np.exp(-0.3 * l))
        u = rmsnorm(h, attn_norm_g[l])
        proj = jnp.einsum('bld,de->ble', u, w_in[l])
        rq, rk, rv, rg, dq, dk, dv = jnp.split(proj, IN_OFFSETS, axis=-1)
        heads = lambda t, nh: t.reshape(b, L, nh, -1).transpose(0, 2, 1, 3)
        pair = lambda t: t.reshape(b, L, DIFF_HEADS, 2, DIFF_QK).transpose(0, 3, 2, 1, 4)
        y_ret = retention(heads(rq, RET_HEADS), heads(rk, RET_HEADS), heads(rv, RET_HEADS), rg, pos, valid)
        y_diff = diff_attention(pair(dq), pair(dk), heads(dv, DIFF_HEADS), pos, valid,
                                lambda_q1[l], lambda_k1[l], lambda_q2[l], lambda_k2[l],
                                diff_subln_g[l], lambda_init)
        mixed = jnp.concatenate([y_ret, y_diff], axis=-1)
        h = h + row_mask * jnp.einsum('ble,ed->bld', mixed, w_out[l])
        u = rmsnorm(h, ffn_norm_g[l]).reshape(b * L, d)
        f = hier_moe(u, w_group_router[l], w_expert_router[l], w_gate[l], w_up[l], w_down[l])
        h = h + row_mask * f.reshape(b, L, d)

    return rmsnorm(h, final_norm_g)[:, CHUNK:, :]
```

```python
import os
import numpy as np
import ml_dtypes
import concourse.bass as bass
import concourse.mybir as mybir
from concourse.bass_utils import run_bass_kernel_spmd

F32 = mybir.dt.float32
BF16 = mybir.dt.bfloat16
I32 = mybir.dt.int32
AF = mybir.ActivationFunctionType
ALU = mybir.AluOpType
AX = mybir.AxisListType

P = 128
D = 2048
KC = 16
NO = 17
NQ = 16
EPS = 1e-6
NEXP = 64
FF = 512
CAP = 128
OVC = 128
XROWS = 8448
RW = D + 3

DEBUG = os.environ.get("MK_DEBUG", "")


class Op:
    __slots__ = ("eng", "fn", "deps", "signal", "signo", "dma", "dval", "noinst")


class Sched:
    ENGS = ("sync", "scalar", "vector", "gpsimd", "tensor")

    def __init__(self):
        self.ops = {e: [] for e in self.ENGS}
        self.dma_count = []
        self.last_dma = {}

    def new_dma_sem(self):
        self.dma_count.append(0)
        return len(self.dma_count) - 1

    def add(self, eng, fn, deps=(), noinst=False):
        op = Op()
        op.noinst = noinst
        op.eng = eng
        op.fn = fn
        op.deps = [d for d in deps if d is not None]
        op.signal = False
        op.signo = 0
        op.dma = None
        op.dval = 0
        for d in op.deps:
            if d.dma is None:
                d.signal = True
        self.ops[eng].append(op)
        return op

    def dma(self, eng, fn, sem_id, deps=(), chain=True):
        deps = list(deps)
        if chain and sem_id in self.last_dma:
            deps.append(self.last_dma[sem_id])
        op = self.add(eng, fn, deps)
        op.dma = sem_id
        self.dma_count[sem_id] += 16
        op.dval = self.dma_count[sem_id]
        self.last_dma[sem_id] = op
        return op

    def barrier(self):
        lasts = []
        for e in ("scalar", "vector", "gpsimd", "tensor"):
            for op in reversed(self.ops[e]):
                if op.dma is None and not op.noinst:
                    lasts.append(op)
                    break
        deps = lasts + list(self.last_dma.values())
        for e in self.ENGS:
            self.add(e, lambda x: None, deps=deps, noinst=True)

    def emit(self, nc, block, esems, dsems):
        for e in self.ENGS:
            cnt = 0
            for op in self.ops[e]:
                if op.dma is None and op.signal:
                    cnt += 1
                    op.signo = cnt
        sched = self

        def make(ename):
            def body(engine):
                waited = {}
                for op in sched.ops[ename]:
                    for d in op.deps:
                        if d.dma is not None:
                            key, val, sem = ("d", d.dma), d.dval, dsems[d.dma]
                        else:
                            key, val, sem = ("e", d.eng), d.signo, esems[d.eng]
                        if waited.get(key, 0) >= val:
                            continue
                        engine.wait_ge(sem, val)
                        waited[key] = val
                    inst = op.fn(engine)
                    if inst is None:
                        continue
                    if op.dma is not None:
                        inst.then_inc(dsems[op.dma], 16)
                    elif op.signal:
                        inst.then_inc(esems[ename], 1)
            return body

        block.sync(make("sync"))
        block.scalar(make("scalar"))
        block.vector(make("vector"))
        block.gpsimd(make("gpsimd"))
        block.tensor(make("tensor"))


class Arena:
    def __init__(self, nc, base=16640, limit=229376):
        self.nc = nc
        self.off = base
        self.limit = limit
        self.n = 0

    def alloc(self, name, shape, dt):
        esz = 2 if dt == BF16 else 4
        size = int(np.prod(shape[1:])) * esz
        size = (size + 63) // 64 * 64
        self.n += 1
        t = self.nc.alloc_sbuf_tensor_at(f"{name}_{self.n}", list(shape), dt, offset=self.off)
        self.off += size
        assert self.off <= self.limit, (name, self.off)
        return t


_REGS = {}


def getreg(g, val):
    key = (id(g), val)
    if key not in _REGS:
        _REGS[key] = g.to_reg(val)
    return _REGS[key]


class Rot:
    def __init__(self, n):
        self.n = n
        self.i = -1
        self.readers = [[] for _ in range(n)]

    def next(self):
        self.i = (self.i + 1) % self.n
        r = self.readers[self.i]
        self.readers[self.i] = []
        return self.i, r

    def read(self, idx, tok):
        self.readers[idx].append(tok)


GAM = [1.0 - 2.0 ** (-5 - h) for h in range(4)]
CDEC = [g ** 128 for g in GAM]
QSCALE = 128 ** -0.5
NEG = -30000.0
TO_W = 768
TQ_W = 1280
CT_W = 512 + 4 + 512 + 256
LAM_INIT = 0.8 - 0.6 * 1.0


def build_program(dbg=""):
    _REGS.clear()
    nc = bass.Bass("TRN2", target_bir_lowering=False)
    S = Sched()
    AR = Arena(nc)

    def din(name, shape, dt=F32):
        return nc.dram_tensor(name, list(shape), dt, kind="ExternalInput").ap()

    def dscratch(name, shape, dt, out=False):
        kind = "ExternalOutput" if out else "Internal"
        return nc.dram_tensor(name, list(shape), dt, kind=kind).ap()

    xo = din("xo", [NO * P, D])
    xq = din("xq", [NQ * P, D])
    w_in = din("w_in", [D, 6144])
    g_attn = din("g_attn", [P, D])
    tab_o = din("tab_o", [NO, P, TO_W])
    tab_q = din("tab_q", [NQ, P, TQ_W])
    ctab_d = din("ctab", [P, CT_W])
    mtab_d = din("mtab", [P, 3 * P], BF16)

    po = dscratch("po", [NO * P, 3584], BF16, out=("p1" in dbg))
    pq = dscratch("pq", [NQ * P, 6144], BF16, out=("p1" in dbg))
    mixd = dscratch("mixd", [NQ * P, D], BF16, out=("p2" in dbg))
    w_out = din("w_out", [D, D])
    wr_d = din("wr", [D, 72])
    g_ffn = din("g_ffn", [P, D])
    g_fin = din("g_fin", [P, D])
    rtab_d = din("rtab", [P, 80])
    utab_d = din("utab", [P, 2 * P], BF16)
    full = dbg in ("", "full")
    if full:
        w_gate = din("w_gate", [NEXP, D, FF])
        w_up = din("w_up", [NEXP, D, FF])
        w_down = din("w_down", [NEXP, FF, D])
    xs = dscratch("xs", [XROWS, RW], F32, out=(dbg in ("p3", "full")))
    h2d = dscratch("h2d", [NQ * P, D], F32, out=(dbg in ("p3", "full")))
    ypair = dscratch("ypair", [2 * NQ * P, RW], F32, out=(dbg == "full"))
    if dbg in ("", "full"):
        out = nc.dram_tensor("out", [NQ * P, D], F32, kind="ExternalOutput").ap()

    from contextlib import ExitStack
    with ExitStack() as es:
        banks = [es.enter_context(nc.psum_tensor(f"bank{i}", [P, 512], F32)) for i in range(8)]
        esems = {e: es.enter_context(nc.semaphore(f"es_{e}")) for e in Sched.ENGS}
        bank_last = [[] for _ in range(8)]

        ident = AR.alloc("ident", [P, P], BF16)
        identf = AR.alloc("identf", [P, P], F32)
        epst = AR.alloc("epst", [P, 1], F32)
        t_ones = S.add("gpsimd", lambda g: g.memset(ident[:], 1.0))
        t_ident = S.add("gpsimd", lambda g: g.affine_select(out=ident[:], in_=ident[:], pattern=[[-1, P]],
                                                            compare_op=ALU.is_equal, fill=0.0, base=0,
                                                            channel_multiplier=1), deps=[t_ones])
        t_onesf = S.add("gpsimd", lambda g: g.memset(identf[:], 1.0))
        t_identf = S.add("gpsimd", lambda g: g.affine_select(out=identf[:], in_=identf[:], pattern=[[-1, P]],
                                                             compare_op=ALU.is_equal, fill=0.0, base=0,
                                                             channel_multiplier=1), deps=[t_onesf])
        t_eps = S.add("vector", lambda v: v.memset(epst[:], EPS))
        phase_mark = AR.off

        gat = AR.alloc("gat", [P, D], F32)
        ssq = AR.alloc("ssq", [P, 64], F32)
        rstd = AR.alloc("rstd", [P, 64], F32)
        junk = AR.alloc("junk", [P, D], BF16)
        xt = [AR.alloc(f"xt{i}", [P, D], F32) for i in range(2)]
        xn = [AR.alloc(f"xn{i}", [P, D], BF16) for i in range(2)]
        uT = AR.alloc("uT", [P, KC, NO * P], BF16)
        wbuf = [AR.alloc(f"wbuf{i}", [P, KC, 512], BF16) for i in range(3)]
        stg = [AR.alloc(f"stg{i}", [P, NO, 512], BF16) for i in range(2)]

        sem_c = S.new_dma_sem()
        t_gat = S.dma("sync", lambda q: q.dma_start(out=gat[:], in_=g_attn), sem_c)
        zt = AR.alloc("zt", [P, 2 * RW], F32)
        t_zt = S.add("gpsimd", lambda g: g.memset(zt[:], 0.0))
        sem_zf = S.new_dma_sem()
        zf_list = [(xs, r) for r in range(XROWS // 256)] + [(ypair, r) for r in range(2 * NQ * P // 256)]

        def zero_fill(n):
            for _ in range(n):
                if not zf_list:
                    return
                dst, r = zf_list.pop(0)
                S.dma("sync", lambda q, dst=dst, r=r: q.dma_start(
                    out=dst[r * 256:(r + 1) * 256, :].rearrange("(p a) c -> p (a c)", a=2), in_=zt[:]),
                    sem_zf, deps=[t_zt], chain=False)
        t_z = S.add("vector", lambda v: v.memset(ssq[:], 0.0))
        sem_x = [S.new_dma_sem() for _ in range(2)]
        sem_w = [S.new_dma_sem() for _ in range(3)]
        sem_st = [S.new_dma_sem() for _ in range(2)]
        xrot = Rot(2)
        xnrot = Rot(2)
        wrot = Rot(3)
        strot = Rot(2)
        uT_readers = [[] for _ in range(NO)]
        uT_ready = [None] * NO

        def norm_chunk(src_ap, ci, ncol):
            xi, rd = xrot.next()
            t_ld = S.dma("sync", lambda q: q.dma_start(out=xt[xi][:], in_=src_ap), sem_x[xi], deps=rd)
            t_sq = S.add("scalar", lambda a: a.activation(out=junk[:], in_=xt[xi][:], func=AF.Square,
                                                          accum_out=ssq[:, ncol:ncol + 1]), deps=[t_ld, t_z])
            t_r1 = S.add("scalar", lambda a: a.activation(out=rstd[:, ncol:ncol + 1], in_=ssq[:, ncol:ncol + 1],
                                                          func=AF.Sqrt, bias=epst[:, 0:1], scale=1.0 / D),
                         deps=[t_sq, t_eps])
            t_r2 = S.add("vector", lambda v: v.reciprocal(out=rstd[:, ncol:ncol + 1], in_=rstd[:, ncol:ncol + 1]),
                         deps=[t_r1])
            ni, nrd = xnrot.next()
            t_xn = S.add("vector", lambda v: v.scalar_tensor_tensor(out=xn[ni][:], in0=xt[xi][:],
                                                                   scalar=rstd[:, ncol:ncol + 1], in1=gat[:],
                                                                   op0=ALU.mult, op1=ALU.mult),
                         deps=[t_r2, t_gat] + nrd)
            xrot.read(xi, t_xn)
            last = []
            for half in range(2):
                bk = half
                pb = banks[bk][:].bitcast(BF16)

                def tr(pe, half=half, pb=pb):
                    inst = None
                    for k in range(8):
                        c = half * 8 + k
                        inst = pe.transpose(out=pb[:, k * P:(k + 1) * P], in_=xn[ni][:, c * P:(c + 1) * P],
                                            identity=ident[:])
                    return inst
                t_tr = S.add("tensor", tr, deps=[t_xn, t_ident] + bank_last[bk])
                eng = "scalar" if half == 0 else "vector"

                def ev(e, half=half, pb=pb, eng=eng):
                    o = uT[:, half * 8:(half + 1) * 8, ci * P:(ci + 1) * P]
                    i = pb.rearrange("p (k t) -> p k t", k=8)
                    if eng == "scalar":
                        return e.copy(out=o, in_=i)
                    return e.tensor_copy(out=o, in_=i)
                t_ev = S.add(eng, ev, deps=[t_tr] + uT_readers[ci])
                bank_last[bk] = [t_ev]
                last.append(t_ev)
                xnrot.read(ni, t_tr)
            uT_readers[ci] = []
            uT_ready[ci] = last

        pb_ctr = [0]

        def gemm(nchunks, blocks, dst):
            for j, blk in enumerate(blocks):
                wi, wrd = wrot.next()
                src = w_in[:, blk * 512:(blk + 1) * 512].rearrange("(c p) n -> p c n", p=P)
                t_w = S.dma("gpsimd", lambda g, wi=wi, src=src: g.dma_start(out=wbuf[wi][:], in_=src), sem_w[wi],
                            deps=wrd)
                si, srd = strot.next()
                evs = []
                for c in range(nchunks):
                    bk = 2 + (pb_ctr[0] % 6)
                    pb_ctr[0] += 1

                    def mm(pe, c=c, bk=bk, wi=wi):
                        inst = None
                        for k in range(KC):
                            inst = pe.matmul(out=banks[bk][:], lhsT=uT[:, k, c * P:(c + 1) * P], rhs=wbuf[wi][:, k, :],
                                             start=(k == 0), stop=(k == KC - 1))
                        return inst
                    t_mm = S.add("tensor", mm, deps=[t_w] + bank_last[bk] + uT_ready[c])
                    uT_readers[c].append(t_mm)
                    wrot.read(wi, t_mm)
                    eng = "scalar" if (c % 2 == 0) else "vector"

                    def ev(e, c=c, bk=bk, si=si, eng=eng):
                        if eng == "scalar":
                            return e.copy(out=stg[si][:, c, :], in_=banks[bk][:])
                        return e.tensor_copy(out=stg[si][:, c, :], in_=banks[bk][:])
                    t_ev = S.add(eng, ev, deps=[t_mm] + (srd if c < 2 else []))
                    bank_last[bk] = [t_ev]
                    evs.append(t_ev)
                d = dst[:, j * 512:(j + 1) * 512].rearrange("(c p) n -> p c n", p=P)
                t_st = S.dma("scalar", lambda q, si=si, d=d, n=nchunks: q.dma_start(out=d, in_=stg[si][:, 0:n, :]),
                             sem_st[si], deps=evs)
                strot.read(si, t_st)

        for c in range(NO):
            norm_chunk(xo[c * P:(c + 1) * P, :], c, c)
            if c >= 3:
                zero_fill(2)
        gemm(NO, [1, 2, 3, 8, 9, 10, 11], po)
        for c in range(NQ):
            norm_chunk(xq[c * P:(c + 1) * P, :], c, NO + c)
            zero_fill(2)
        zero_fill(100)
        gemm(NQ, list(range(12)), pq)
        S.barrier()
        bank_last = [[] for _ in range(8)]

        if "p1" not in dbg:
            AR.off = phase_mark
            KT = AR.alloc("KT", [P, 8, 33 * P], BF16)
            VV = AR.alloc("VV", [P, 33, 4, 257], BF16)
            pqt = AR.alloc("pqt", [P, 5120], BF16)
            pot = AR.alloc("pot", [P, 2560], BF16)
            tq = AR.alloc("tq", [P, TQ_W], F32)
            to = AR.alloc("to", [P, TO_W], F32)
            ctab = AR.alloc("ctab", [P, CT_W], F32)
            mtab = AR.alloc("mtab", [P, 3 * P], BF16)
            t1 = AR.alloc("t1", [P, 512], F32)
            t2 = AR.alloc("t2", [P, 512], F32)
            g1 = AR.alloc("g1", [P, 256], F32)
            g2 = AR.alloc("g2", [P, 256], F32)
            kro = AR.alloc("kro", [P, 4, P], BF16)
            krq = AR.alloc("krq", [P, 4, P], BF16)
            qr = AR.alloc("qr", [P, 4, P], BF16)
            kqT = AR.alloc("kqT", [P, 8, P], BF16)
            scT = AR.alloc("scT", [P, 4, P], BF16)
            state = AR.alloc("state", [P, 4, 256], F32)
            stbf = AR.alloc("stbf", [P, 4, 256], BF16)
            stmp = AR.alloc("stmp", [P, 4, 256], F32)
            QT = AR.alloc("QT", [P, 8, P], BF16)
            PT = [AR.alloc(f"PT{i}", [P, 512], BF16) for i in range(4)]
            mixed = AR.alloc("mixed", [P, D], BF16)
            ro = AR.alloc("ro", [P, 1024], F32)
            gs = AR.alloc("gs", [P, 1024], F32)
            sm = AR.alloc("sm", [P, 32], F32)
            dtmp = AR.alloc("dtmp", [P, 256], F32)
            dtmp2 = AR.alloc("dtmp2", [P, 256], F32)
            gsub = AR.alloc("gsub", [P, 256], F32)
            junk2 = AR.alloc("junk2", [P, 256], BF16)

            def VE(fn, deps=()):
                return S.add("vector", fn, deps)

            def AC(fn, deps=()):
                return S.add("scalar", fn, deps)

            def GP(fn, deps=()):
                return S.add("gpsimd", fn, deps)

            def PE(fn, deps=()):
                return S.add("tensor", fn, deps)

            sem_k = S.new_dma_sem()
            t_ct = S.dma("sync", lambda q: q.dma_start(out=ctab[:], in_=ctab_d), sem_k)
            sem_k2 = S.new_dma_sem()
            t_mt = S.dma("sync", lambda q: q.dma_start(out=mtab[:], in_=mtab_d), sem_k2)
            tri4 = ctab[:, 0:512]
            xit = ctab[:, 512:516]
            lamv = ctab[:, 516:1028]
            sgv = ctab[:, 1028:1284]
            t_l1 = VE(lambda v: v.tensor_tensor(out=t1[:, 0:128], in0=lamv[:, 0:128], in1=lamv[:, 128:256], op=ALU.mult), [t_ct])
            t_l2 = VE(lambda v: v.reduce_sum(out=sm[:, 0:1], in_=t1[:, 0:128], axis=AX.X), [t_l1])
            t_l3 = VE(lambda v: v.tensor_tensor(out=t1[:, 128:256], in0=lamv[:, 256:384], in1=lamv[:, 384:512], op=ALU.mult), [t_ct])
            t_l4 = VE(lambda v: v.reduce_sum(out=sm[:, 1:2], in_=t1[:, 128:256], axis=AX.X), [t_l3])
            t_l5 = AC(lambda a: a.activation(out=sm[:, 2:4], in_=sm[:, 0:2], func=AF.Exp), [t_l2, t_l4])
            t_l6 = VE(lambda v: v.tensor_tensor(out=sm[:, 4:5], in0=sm[:, 3:4], in1=sm[:, 2:3], op=ALU.subtract), [t_l5])
            t_lam = VE(lambda v: v.tensor_scalar(out=sm[:, 5:6], in0=sm[:, 4:5], scalar1=-LAM_INIT, scalar2=1.0, op0=ALU.add,
                                                 op1=ALU.mult), [t_l6])
            t_gsub = VE(lambda v: v.tensor_scalar(out=gsub[:], in0=sgv, scalar1=1.0 - LAM_INIT, scalar2=0.0, op0=ALU.mult,
                                                  op1=ALU.add), [t_ct])
            t_st0 = VE(lambda v: v.memset(state[:], 0.0))
            t_st1 = VE(lambda v: v.memset(stbf[:], 0.0))
            t_vones = GP(lambda g: g.memset(VV[:, :, :, 256:257], 1.0))

            sem_po = S.new_dma_sem()
            sem_to = S.new_dma_sem()
            sem_vo = S.new_dma_sem()
            sem_pq = S.new_dma_sem()
            sem_tq = S.new_dma_sem()
            sem_vq = S.new_dma_sem()
            sem_mx = S.new_dma_sem()
            p01 = [0]
            qkc = [0]
            st = {"prev_last": [], "stbf_readers": [], "qt_readers": [], "mix_dma": None, "vv": [t_vones], "kt": [],
                  "state_tok": [t_st0, t_st1]}

            def engine_marks():
                return {e: len(S.ops[e]) for e in ("scalar", "vector", "gpsimd", "tensor")}

            def last_since(marks):
                res = []
                for e, n0 in marks.items():
                    for op in reversed(S.ops[e][n0:]):
                        if op.dma is None and not op.noinst:
                            res.append(op)
                            break
                return res

            def ret_rope(src, ctab_ap, stab_ap, dst, deps):
                sv = src.rearrange("p (h t f) -> p h t f", h=4, t=2)
                x1 = sv[:, :, 0, :]
                x2 = sv[:, :, 1, :]
                cv = ctab_ap.rearrange("p (h f) -> p h f", h=4)
                svt = stab_ap.rearrange("p (h f) -> p h f", h=4)
                ta = t1[:, 0:256].rearrange("p (h f) -> p h f", h=4)
                tb = t1[:, 256:512].rearrange("p (h f) -> p h f", h=4)
                tc = t2[:, 0:256].rearrange("p (h f) -> p h f", h=4)
                td = t2[:, 256:512].rearrange("p (h f) -> p h f", h=4)
                a = VE(lambda v: v.tensor_tensor(out=ta, in0=x1, in1=cv, op=ALU.mult), deps)
                b = VE(lambda v: v.tensor_tensor(out=tb, in0=x2, in1=svt, op=ALU.mult), deps)
                c = VE(lambda v: v.tensor_tensor(out=tc, in0=x1, in1=svt, op=ALU.mult), deps)
                d = VE(lambda v: v.tensor_tensor(out=td, in0=x2, in1=cv, op=ALU.mult), deps)
                o1 = VE(lambda v: v.tensor_tensor(out=dst[:, :, 0:64], in0=ta, in1=tb, op=ALU.subtract), [a, b] + list(deps))
                o2 = VE(lambda v: v.tensor_tensor(out=dst[:, :, 64:128], in0=tc, in1=td, op=ALU.add), [c, d] + list(deps))
                return [o1, o2]

            def diff_rope(blk, cos_ap, sin_ap, deps):
                bv = blk.rearrange("p (c d) -> p c d", c=8)
                x1 = bv[:, :, 0:16]
                x2 = bv[:, :, 16:32]
                cv = cos_ap.rearrange("p (c f) -> p c f", c=8)
                sv = sin_ap.rearrange("p (c f) -> p c f", c=8)
                ta = g1[:, 0:128].rearrange("p (c f) -> p c f", c=8)
                tb = g1[:, 128:256].rearrange("p (c f) -> p c f", c=8)
                tc = g2[:, 0:128].rearrange("p (c f) -> p c f", c=8)
                td = g2[:, 128:256].rearrange("p (c f) -> p c f", c=8)
                a = GP(lambda g: g.tensor_tensor(out=ta, in0=x1, in1=cv, op=ALU.mult), deps)
                b = GP(lambda g: g.tensor_tensor(out=tb, in0=x2, in1=sv, op=ALU.mult), deps)
                c = GP(lambda g: g.tensor_tensor(out=tc, in0=x1, in1=sv, op=ALU.mult), deps)
                d = GP(lambda g: g.tensor_tensor(out=td, in0=x2, in1=cv, op=ALU.mult), deps)
                o1 = GP(lambda g: g.tensor_tensor(out=x1, in0=ta, in1=tb, op=ALU.subtract), [a, b, c, d])
                o2 = GP(lambda g: g.tensor_tensor(out=x2, in0=tc, in1=td, op=ALU.add), [a, b, c, d, o1])
                return [o1, o2]

            def transpose8(srcs, dst, deps, wdeps):
                bk = p01[0] % 2
                p01[0] += 1
                pb = banks[bk][:].bitcast(BF16)

                def tr(pe):
                    inst = None
                    for k, sap in enumerate(srcs):
                        inst = pe.transpose(out=pb[:, k * P:(k + 1) * P], in_=sap, identity=ident[:])
                    return inst
                t_tr = PE(tr, list(deps) + bank_last[bk])
                t_ev = AC(lambda a: a.copy(out=dst, in_=pb.rearrange("p (k t) -> p k t", k=8)), [t_tr] + list(wdeps))
                bank_last[bk] = [t_ev]
                return t_tr, t_ev

            def state_update(kmat, rv_ap, deps):
                def mm(pe):
                    inst = None
                    for h in range(4):
                        inst = pe.matmul(out=banks[6 + h // 2][:, (h % 2) * 256:(h % 2 + 1) * 256], lhsT=kmat[:, h, :],
                                         rhs=rv_ap[:, h * 256:(h + 1) * 256], start=True, stop=True)
                    return inst
                t_kv = PE(mm, list(deps) + bank_last[6] + bank_last[7])
                adds = []
                for hb in range(2):
                    adds.append(VE(lambda v, hb=hb: v.tensor_tensor(
                        out=stmp[:, 2 * hb:2 * hb + 2, :], in0=banks[6 + hb][:].rearrange("p (h e) -> p h e", h=2),
                        in1=state[:, 2 * hb:2 * hb + 2, :], op=ALU.add), [t_kv] + st["state_tok"]))
                bank_last[6] = [adds[0]]
                bank_last[7] = [adds[1]]
                ups = []
                for h in range(4):
                    ups.append(VE(lambda v, h=h: v.tensor_scalar(out=state[:, h, :], in0=stmp[:, h, :], scalar1=CDEC[h],
                                                                 scalar2=0.0, op0=ALU.mult, op1=ALU.add), adds))
                t_bf = AC(lambda a: a.copy(out=stbf[:], in_=state[:]), ups + st["stbf_readers"])
                st["stbf_readers"] = []
                st["state_tok"] = ups + [t_bf]
                return t_kv, t_bf

            def prep_other(i):
                rows = po[i * P:(i + 1) * P, :]
                pl = st["prev_last"]
                t_a = S.dma("sync", lambda q: q.dma_start(out=pot[:], in_=rows[:, 0:2560]), sem_po, deps=pl)
                t_b = S.dma("sync", lambda q: q.dma_start(out=to[:], in_=tab_o[i]), sem_to, deps=pl)
                t_v = S.dma("sync", lambda q: q.dma_start(out=VV[:, i, :, 0:256],
                                                          in_=rows[:, 2560:3584].rearrange("p (h e) -> p h e", h=4)), sem_vo,
                            deps=[t_vones])
                st["vv"].append(t_v)
                rk = ret_rope(pot[:, 0:512], to[:, 0:256], to[:, 256:512], kro, [t_a, t_b] + pl)
                dr = diff_rope(pot[:, 1536:2560], to[:, 512:640], to[:, 640:768], [t_a, t_b] + pl)
                return rk, dr

            def prep_other_b(i, rk, dr):
                t_kv, t_bf = state_update(kro, pot[:, 512:1536], rk)
                dkv = pot[:, 1536:2560].rearrange("p (c d) -> p c d", c=8)
                t_tr, t_ev = transpose8([dkv[:, c, :] for c in range(8)], KT[:, :, i * P:(i + 1) * P], dr, [])
                st["kt"].append(t_ev)

            def prep_own_a(j):
                rows = pq[j * P:(j + 1) * P, :]
                pl = st["prev_last"]
                t_a = S.dma("sync", lambda q: q.dma_start(out=pqt[:], in_=rows[:, 0:5120]), sem_pq, deps=pl)
                t_b = S.dma("sync", lambda q: q.dma_start(out=tq[:], in_=tab_q[j]), sem_tq, deps=pl)
                t_v = S.dma("sync", lambda q: q.dma_start(out=VV[:, NO + j, :, 0:256],
                                                          in_=rows[:, 5120:6144].rearrange("p (h e) -> p h e", h=4)), sem_vq,
                            deps=[t_vones])
                st["vv"].append(t_v)
                ld = [t_a, t_b] + pl
                rk = ret_rope(pqt[:, 512:1024], tq[:, 0:256], tq[:, 256:512], krq, ld)
                rq = ret_rope(pqt[:, 0:512], tq[:, 512:768], tq[:, 768:1024], qr, ld)
                dk = diff_rope(pqt[:, 4096:5120], tq[:, 1024:1152], tq[:, 1152:1280], ld)
                dq = diff_rope(pqt[:, 3072:4096], tq[:, 1024:1152], tq[:, 1152:1280], ld)
                t_s0 = AC(lambda a: a.activation(out=gs[:], in_=pqt[:, 2048:3072], func=AF.Exp, scale=-1.0), ld)
                t_s1 = VE(lambda v: v.tensor_scalar(out=gs[:], in0=gs[:], scalar1=1.0, scalar2=1.0, op0=ALU.add,
                                                    op1=ALU.mult), [t_s0])
                t_s2 = VE(lambda v: v.reciprocal(out=gs[:], in_=gs[:]), [t_s1])
                t_silu = VE(lambda v: v.tensor_tensor(out=gs[:], in0=gs[:], in1=pqt[:, 2048:3072], op=ALU.mult), [t_s2])
                return dict(rk=rk, rq=rq, dk=dk, dq=dq, silu=t_silu, ld=ld)

            def prep_own_b(j, pa):
                t_tr, t_ev = transpose8([krq[:, h, :] for h in range(4)] + [qr[:, h, :] for h in range(4)], kqT[:],
                                        pa["rk"] + pa["rq"], st["prev_last"])
                bk = p01[0] % 2
                p01[0] += 1

                def sc(pe):
                    inst = None
                    for h in range(4):
                        inst = pe.matmul(out=banks[bk][:, h * P:(h + 1) * P], lhsT=kqT[:, h, :], rhs=kqT[:, 4 + h, :],
                                         start=True, stop=True)
                    return inst
                t_sc = PE(sc, [t_ev] + bank_last[bk])
                t_scT = VE(lambda v: v.tensor_tensor(out=scT[:].rearrange("p h c -> p (h c)"), in0=banks[bk][:], in1=tri4,
                                                     op=ALU.mult), [t_sc, t_ct] + st["prev_last"])
                bank_last[bk] = [t_scT]

                def om(pe):
                    inst = None
                    for h in range(4):
                        o = banks[4 + h // 2][:, (h % 2) * 256:(h % 2 + 1) * 256]
                        pe.matmul(out=o, lhsT=scT[:, h, :], rhs=pqt[:, 1024 + h * 256:1024 + (h + 1) * 256], start=True,
                                  stop=False)
                        inst = pe.matmul(out=o, lhsT=kqT[:, 4 + h, :], rhs=stbf[:, h, :], start=False, stop=True)
                    return inst
                t_om = PE(om, [t_scT] + st["state_tok"] + bank_last[4] + bank_last[5])
                st["stbf_readers"].append(t_om)
                t_z = VE(lambda v: v.memset(sm[:, 8:12], 0.0), st["prev_last"])
                cps = []
                for h in range(4):
                    cps.append(AC(lambda a, h=h: a.activation(out=ro[:, h * 256:(h + 1) * 256],
                                                              in_=banks[4 + h // 2][:, (h % 2) * 256:(h % 2 + 1) * 256],
                                                              func=AF.Copy, scale=xit[:, h:h + 1]), [t_om, t_ct] + st["prev_last"]))
                bank_last[4] = [cps[1]]
                bank_last[5] = [cps[3]]
                sqs = []
                for h in range(4):
                    sqs.append(AC(lambda a, h=h: a.activation(out=junk2[:], in_=ro[:, h * 256:(h + 1) * 256], func=AF.Square,
                                                              accum_out=sm[:, 8 + h:9 + h]), [cps[h], t_z]))
                t_sq = AC(lambda a: a.activation(out=sm[:, 24:28], in_=sm[:, 8:12], func=AF.Ln, bias=epst[:, 0:1],
                                                 scale=1.0 / 256), sqs + [t_eps])
                t_rc = AC(lambda a: a.activation(out=sm[:, 12:16], in_=sm[:, 24:28], func=AF.Exp, scale=-0.5), [t_sq])
                t_gm = VE(lambda v: v.tensor_tensor(out=gs[:], in0=gs[:], in1=ro[:], op=ALU.mult), [pa["silu"]] + cps)
                mdeps = [t_gm, t_rc] + ([st["mix_dma"]] if st["mix_dma"] is not None else [])
                mret = []
                for h in range(4):
                    mret.append(VE(lambda v, h=h: v.tensor_scalar_mul(out=mixed[:, h * 256:(h + 1) * 256],
                                                                     in0=gs[:, h * 256:(h + 1) * 256],
                                                                     scalar1=sm[:, 12 + h:13 + h]), mdeps))
                state_update(krq, pqt[:, 1024:2048], pa["rk"] + [t_om])
                dkv = pqt[:, 4096:5120].rearrange("p (c d) -> p c d", c=8)
                t_tr, t_ev = transpose8([dkv[:, c, :] for c in range(8)], KT[:, :, (NO + j) * P:(NO + j + 1) * P], pa["dk"], [])
                st["kt"].append(t_ev)
                dqv = pqt[:, 3072:4096].rearrange("p (c d) -> p c d", c=8)
                t_tr2, t_ev2 = transpose8([dqv[:, c, :] for c in range(8)], QT[:], pa["dq"], st["qt_readers"])
                st["qt_readers"] = []
                return mret, t_ev2

            def attn(j, mret, t_qt):
                tiles = [(i, (0 if i == 0 else (1 if i == 1 else None))) for i in range(j + 2)]
                tiles += [(NO + jj, (2 if jj == j else None)) for jj in range(j + 1)]
                groups = []
                for h in range(4):
                    for comp in range(2):
                        hc = 2 * h + comp
                        ob = (2 if h % 2 == 0 else 4) + comp
                        for g0 in range(0, len(tiles), 4):
                            groups.append(dict(h=h, comp=comp, hc=hc, ob=ob, tl=tiles[g0:g0 + 4], first=(g0 == 0),
                                               last=(g0 + 4 >= len(tiles))))
                kv_ready = st["kt"] + st["vv"]
                st["kt"] = []
                st["vv"] = []

                def emit_qk(G):
                    bk = (0, 1, 6, 7)[qkc[0] % 4]
                    qkc[0] += 1
                    G["bk"] = bk

                    def qk(pe, G=G, bk=bk):
                        inst = None
                        for g, (slot, mk) in enumerate(G["tl"]):
                            o = banks[bk][:, g * P:(g + 1) * P]
                            inst = pe.matmul(out=o, lhsT=KT[:, G["hc"], slot * P:(slot + 1) * P], rhs=QT[:, G["hc"], :],
                                             start=True, stop=(mk is None))
                            if mk is not None:
                                inst = pe.matmul(out=o, lhsT=ident[:], rhs=mtab[:, mk * P:(mk + 1) * P], start=False, stop=True)
                        return inst
                    G["t_qk"] = PE(qk, [t_qt, t_mt, t_ident] + kv_ready + bank_last[bk])
                    st["qt_readers"].append(G["t_qk"])

                pt_readers = [[], [], [], []]
                for n0 in range(min(3, len(groups))):
                    emit_qk(groups[n0])
                posts = []
                for n, G in enumerate(groups):
                    if n + 3 < len(groups):
                        emit_qk(groups[n + 3])
                    pi = n % 4
                    nn = len(G["tl"])
                    t_ex = AC(lambda a, G=G, pi=pi, nn=nn: a.activation(out=PT[pi][:, 0:nn * P], in_=banks[G["bk"]][:, 0:nn * P],
                                                                        func=AF.Exp, scale=QSCALE), [G["t_qk"]] + pt_readers[pi])
                    bank_last[G["bk"]] = [t_ex]
                    pt_readers[pi] = []

                    def av(pe, G=G, pi=pi):
                        inst = None
                        for g, (slot, mk) in enumerate(G["tl"]):
                            inst = pe.matmul(out=banks[G["ob"]][:, 0:257], lhsT=PT[pi][:, g * P:(g + 1) * P],
                                             rhs=VV[:, slot, G["h"], :], start=(G["first"] and g == 0),
                                             stop=(G["last"] and g == len(G["tl"]) - 1))
                        return inst
                    t_av = PE(av, [t_ex] + (bank_last[G["ob"]] if G["first"] else []))
                    pt_readers[pi].append(t_av)
                    if G["last"] and G["comp"] == 1:
                        h = G["h"]
                        ob = G["ob"] - 1
                        O1 = banks[ob]
                        O2 = banks[ob + 1]
                        r1 = VE(lambda v, O1=O1: v.reciprocal(out=sm[:, 16:17], in_=O1[:, 256:257]), [t_av] + posts[-1:])
                        r2 = VE(lambda v, O2=O2: v.reciprocal(out=sm[:, 17:18], in_=O2[:, 256:257]), [t_av] + posts[-1:])
                        r3 = VE(lambda v: v.tensor_tensor(out=sm[:, 18:19], in0=sm[:, 17:18], in1=sm[:, 5:6], op=ALU.mult),
                                [r2, t_lam])
                        z = VE(lambda v: v.memset(sm[:, 19:20], 0.0), posts[-1:])
                        a1 = AC(lambda a, O2=O2: a.activation(out=dtmp[:], in_=O2[:, 0:256], func=AF.Copy, scale=sm[:, 18:19]),
                                [r3] + posts[-1:])
                        d1 = VE(lambda v, O1=O1: v.scalar_tensor_tensor(out=dtmp2[:], in0=O1[:, 0:256], scalar=sm[:, 16:17],
                                                                        in1=dtmp[:], op0=ALU.mult, op1=ALU.add),
                                [a1, r1] + posts[-1:])
                        bank_last[ob] = [d1]
                        bank_last[ob + 1] = [a1]
                        a2 = AC(lambda a: a.activation(out=junk2[:], in_=dtmp2[:], func=AF.Square, accum_out=sm[:, 19:20]),
                                [d1, z])
                        a3 = AC(lambda a: a.activation(out=sm[:, 21:22], in_=sm[:, 19:20], func=AF.Ln, bias=epst[:, 0:1],
                                                       scale=1.0 / 256), [a2])
                        r4 = AC(lambda a: a.activation(out=sm[:, 20:21], in_=sm[:, 21:22], func=AF.Exp, scale=-0.5), [a3])
                        mdeps = [r4, t_gsub, d1] + ([st["mix_dma"]] if st["mix_dma"] is not None else [])
                        fin = VE(lambda v, h=h: v.scalar_tensor_tensor(out=mixed[:, 1024 + h * 256:1024 + (h + 1) * 256],
                                                                      in0=dtmp2[:], scalar=sm[:, 20:21], in1=gsub[:],
                                                                      op0=ALU.mult, op1=ALU.mult), mdeps)
                        posts.append(fin)
                t_out = S.dma("scalar", lambda q: q.dma_start(out=mixd[j * P:(j + 1) * P, :], in_=mixed[:]), sem_mx,
                              deps=posts + mret)
                st["mix_dma"] = t_out

            m0 = engine_marks()
            rk, dr = prep_other(0)
            prep_other_b(0, rk, dr)
            st["prev_last"] = last_since(m0)
            m0 = engine_marks()
            rk, dr = prep_other(1)
            prep_other_b(1, rk, dr)
            st["prev_last"] = last_since(m0)
            m0 = engine_marks()
            pa = prep_own_a(0)
            cur = prep_own_b(0, pa)
            st["prev_last"] = last_since(m0)
            for j in range(NQ):
                m0 = engine_marks()
                oth = None
                if j + 2 < NO:
                    oth = prep_other(j + 2)
                if j + 1 < NQ:
                    pa = prep_own_a(j + 1)
                attn(j, cur[0], cur[1])
                if oth is not None:
                    prep_other_b(j + 2, oth[0], oth[1])
                if j + 1 < NQ:
                    cur = prep_own_b(j + 1, pa)
                st["prev_last"] = last_since(m0)
            S.barrier()
            bank_last = [[] for _ in range(8)]
            if "p2" not in dbg:
                AR.off = phase_mark
                wo = AR.alloc("wo", [P, KC, D], BF16)
                wr = AR.alloc("wr", [P, KC, 72], F32)
                gff = AR.alloc("gff", [P, D], F32)
                rtab = AR.alloc("rtab", [P, 80], F32)
                utab = AR.alloc("utab", [P, 2 * P], BF16)
                mxt = [AR.alloc(f"mxt{i}", [P, D], BF16) for i in range(2)]
                mT = [AR.alloc(f"mT{i}", [P, KC, P], BF16) for i in range(2)]
                xqt = [AR.alloc(f"xqt{i}", [P, D], F32) for i in range(2)]
                h2t = [AR.alloc(f"h2t{i}", [P, D], F32) for i in range(2)]
                rowA = [AR.alloc(f"rowA{i}", [P, RW], F32) for i in range(2)]
                rowB = [AR.alloc(f"rowB{i}", [P, RW], F32) for i in range(2)]
                u2T = AR.alloc("u2T", [P, KC, P], F32)
                lgs = AR.alloc("lgs", [P, 72], F32)
                Oall = AR.alloc("Oall", [P, NQ, 64], BF16)
                OVall = AR.alloc("OVall", [P, NQ, 2], BF16)
                rs = AR.alloc("rs", [P, 64], F32)
                r8 = AR.alloc("r8", [P, 64], F32)
                r64 = AR.alloc("r64", [P, 4, 64], F32)
                idx = [AR.alloc(f"idx{i}", [P, 2], I32) for i in range(2)]
                junk3 = AR.alloc("junk3", [P, D], BF16)
                ssq3 = AR.alloc("ssq3", [P, 32], F32)

                sem_wo = [S.new_dma_sem() for _ in range(4)]
                t_wo = []
                for nb in range(4):
                    t_wo.append(S.dma("gpsimd", lambda g, nb=nb: g.dma_start(
                        out=wo[:, :, nb * 512:(nb + 1) * 512],
                        in_=w_out[:, nb * 512:(nb + 1) * 512].rearrange("(c p) n -> p c n", p=P)), sem_wo[nb]))
                sem_c3 = [S.new_dma_sem() for _ in range(4)]
                t_wr = S.dma("sync", lambda q: q.dma_start(out=wr[:], in_=wr_d.rearrange("(c p) n -> p c n", p=P)), sem_c3[0])
                t_gff = S.dma("sync", lambda q: q.dma_start(out=gff[:], in_=g_ffn), sem_c3[1])
                t_rt = S.dma("sync", lambda q: q.dma_start(out=rtab[:], in_=rtab_d), sem_c3[2])
                t_ut = S.dma("sync", lambda q: q.dma_start(out=utab[:], in_=utab_d), sem_c3[3])
                iota8 = rtab[:, 0:8]
                iota64 = rtab[:, 8:72]
                pid2 = rtab[:, 72:73]
                Umat = utab[:, 0:P]
                Ones = utab[:, P:2 * P]
                t_z3 = VE(lambda v: v.memset(ssq3[:], 0.0))

                sem_mx3 = [S.new_dma_sem() for _ in range(2)]
                sem_xq3 = [S.new_dma_sem() for _ in range(2)]
                sem_h2 = [S.new_dma_sem() for _ in range(2)]
                sem_sc = [S.new_dma_sem() for _ in range(4)]
                rowA_rd = [[], []]
                rowB_rd = [[], []]
                h2_rd = [[], []]
                mxt_rd = [[], []]
                xqt_rd = [[], []]
                mT_rd = [[], []]
                idx_rd = [[], []]
                u2T_rd = []
                O_tok = []
                prev_route = []

                for t in range(NQ):
                    b2 = t % 2
                    t_lm = S.dma("sync", lambda q, t=t, b2=b2: q.dma_start(out=mxt[b2][:], in_=mixd[t * P:(t + 1) * P, :]),
                                 sem_mx3[b2], deps=mxt_rd[b2])
                    t_lx = S.dma("sync", lambda q, t=t, b2=b2: q.dma_start(out=xqt[b2][:], in_=xq[t * P:(t + 1) * P, :]),
                                 sem_xq3[b2], deps=xqt_rd[b2])
                    mxt_rd[b2] = []
                    xqt_rd[b2] = []
                    evs = []
                    for half in range(2):
                        t_tr, t_ev = transpose8([mxt[b2][:, (half * 8 + k) * P:(half * 8 + k + 1) * P] for k in range(8)],
                                                mT[b2][:, half * 8:(half + 1) * 8, :], [t_lm], mT_rd[b2])
                        mxt_rd[b2].append(t_tr)
                        evs.append(t_ev)
                    mT_rd[b2] = []
                    mms = []
                    for nb in range(4):
                        def mm(pe, nb=nb, b2=b2):
                            inst = None
                            for k in range(KC):
                                inst = pe.matmul(out=banks[2 + nb][:], lhsT=mT[b2][:, k, :], rhs=wo[:, k, nb * 512:(nb + 1) * 512],
                                                 start=(k == 0), stop=(k == KC - 1))
                            return inst
                        t_mm = PE(mm, evs + [t_wo[nb]] + bank_last[2 + nb])
                        mT_rd[b2].append(t_mm)
                        mms.append(t_mm)
                    adds = []
                    for nb in range(4):
                        a = VE(lambda v, nb=nb, b2=b2: v.tensor_tensor(out=h2t[b2][:, nb * 512:(nb + 1) * 512], in0=banks[2 + nb][:],
                                                                       in1=xqt[b2][:, nb * 512:(nb + 1) * 512], op=ALU.add),
                               [mms[nb], t_lx] + h2_rd[b2])
                        bank_last[2 + nb] = [a]
                        adds.append(a)
                    h2_rd[b2] = []
                    xqt_rd[b2] = adds[:]
                    t_sh = S.dma("scalar", lambda q, t=t, b2=b2: q.dma_start(out=h2d[t * P:(t + 1) * P, :], in_=h2t[b2][:]), sem_h2[b2],
                                 deps=adds)
                    h2_rd[b2].append(t_sh)
                    t_sq = AC(lambda a, t=t, b2=b2: a.activation(out=junk3[:], in_=h2t[b2][:], func=AF.Square,
                                                                accum_out=ssq3[:, t:t + 1]), adds + [t_z3])
                    t_r1 = AC(lambda a, t=t: a.activation(out=ssq3[:, 16 + t:17 + t], in_=ssq3[:, t:t + 1], func=AF.Sqrt,
                                                          bias=epst[:, 0:1], scale=1.0 / D), [t_sq])
                    t_r2 = VE(lambda v, t=t: v.reciprocal(out=ssq3[:, 16 + t:17 + t], in_=ssq3[:, 16 + t:17 + t]), [t_r1])
                    t_u2 = VE(lambda v, t=t, b2=b2: v.scalar_tensor_tensor(out=rowA[b2][:, 0:D], in0=h2t[b2][:],
                                                                          scalar=ssq3[:, 16 + t:17 + t], in1=gff[:], op0=ALU.mult,
                                                                          op1=ALU.mult), [t_r2, t_gff] + rowA_rd[b2])
                    rowA_rd[b2] = []
                    h2_rd[b2] += [t_u2, t_sq]
                    t_cp = AC(lambda a, b2=b2: a.copy(out=rowB[b2][:, 0:D], in_=rowA[b2][:, 0:D]), [t_u2] + rowB_rd[b2])
                    rowB_rd[b2] = []
                    tre = []
                    for q4 in range(4):
                        bk = p01[0] % 2
                        p01[0] += 1

                        def trf(pe, q4=q4, bk=bk, b2=b2):
                            inst = None
                            for k in range(4):
                                c = q4 * 4 + k
                                inst = pe.transpose(out=banks[bk][:, k * P:(k + 1) * P], in_=rowA[b2][:, c * P:(c + 1) * P],
                                                    identity=identf[:])
                            return inst
                        t_tr = PE(trf, [t_u2, t_identf] + bank_last[bk])
                        t_ev = VE(lambda v, q4=q4, bk=bk: v.tensor_copy(out=u2T[:, q4 * 4:(q4 + 1) * 4, :],
                                                                       in_=banks[bk][:].rearrange("p (k t) -> p k t", k=4)),
                                  [t_tr] + u2T_rd)
                        bank_last[bk] = [t_ev]
                        tre.append(t_ev)
                        rowA_rd[b2].append(t_tr)
                    u2T_rd = []

                    def lg(pe):
                        inst = None
                        for k in range(KC):
                            inst = pe.matmul(out=banks[6][:, 0:72], lhsT=u2T[:, k, :], rhs=wr[:, k, :], start=(k == 0),
                                             stop=(k == KC - 1))
                        return inst
                    t_lg = PE(lg, tre + [t_wr] + bank_last[6])
                    u2T_rd.append(t_lg)
                    c_lg = VE(lambda v: v.tensor_copy(out=lgs[:], in_=banks[6][:, 0:72]), [t_lg] + prev_route)
                    bank_last[6] = [c_lg]
                    gl = lgs[:, 0:8]
                    el = lgs[:, 8:72].rearrange("p (g j) -> p g j", g=8)
                    ohg = r8[:, 0:8]
                    gex = r8[:, 8:16]
                    esel = r8[:, 16:24]
                    oh1 = r8[:, 24:32]
                    es2 = r8[:, 32:40]
                    oh2 = r8[:, 40:48]
                    tmp8 = r8[:, 48:56]

                    def C(i):
                        return rs[:, i:i + 1]
                    ch = [c_lg]

                    def vch(fn, extra=()):
                        o = VE(fn, ch[-1:] + list(extra))
                        ch.append(o)
                        return o

                    def ach(fn, extra=()):
                        o = AC(fn, ch[-1:] + list(extra))
                        ch.append(o)
                        return o
                    vch(lambda v: v.memset(rs[:, 2:3], 0.0))
                    vch(lambda v: v.reduce_max(out=C(0), in_=gl, axis=AX.X))
                    vch(lambda v: v.tensor_scalar(out=ohg, in0=gl, scalar1=C(0), scalar2=0.0, op0=ALU.is_equal, op1=ALU.add))
                    vch(lambda v: v.tensor_scalar(out=C(1), in0=C(0), scalar1=-1.0, scalar2=0.0, op0=ALU.mult, op1=ALU.add))
                    ach(lambda a: a.activation(out=gex, in_=gl, func=AF.Exp, bias=C(1), scale=1.0, accum_out=C(2)))
                    vch(lambda v: v.reciprocal(out=C(3), in_=C(2)))
                    vch(lambda v: v.memset(esel, 0.0))
                    for g in range(8):
                        vch(lambda v, g=g: v.scalar_tensor_tensor(out=esel, in0=el[:, g, :], scalar=ohg[:, g:g + 1], in1=esel,
                                                                  op0=ALU.mult, op1=ALU.add))
                    vch(lambda v: v.reduce_max(out=C(4), in_=esel, axis=AX.X))
                    vch(lambda v: v.tensor_scalar(out=oh1, in0=esel, scalar1=C(4), scalar2=0.0, op0=ALU.is_equal, op1=ALU.add))
                    vch(lambda v: v.scalar_tensor_tensor(out=es2, in0=oh1, scalar=-1e30, in1=esel, op0=ALU.mult, op1=ALU.add))
                    vch(lambda v: v.reduce_max(out=C(5), in_=es2, axis=AX.X))
                    vch(lambda v: v.tensor_scalar(out=oh2, in0=es2, scalar1=C(5), scalar2=0.0, op0=ALU.is_equal, op1=ALU.add))
                    vch(lambda v: v.tensor_tensor(out=C(6), in0=C(5), in1=C(4), op=ALU.subtract))
                    ach(lambda a: a.activation(out=C(7), in_=C(6), func=AF.Exp))
                    vch(lambda v: v.tensor_scalar(out=C(8), in0=C(7), scalar1=1.0, scalar2=0.0, op0=ALU.add, op1=ALU.add))
                    vch(lambda v: v.reciprocal(out=C(8), in_=C(8)))
                    vch(lambda v: v.tensor_tensor(out=C(9), in0=C(7), in1=C(8), op=ALU.mult))
                    vch(lambda v: v.tensor_tensor(out=C(10), in0=C(8), in1=C(3), op=ALU.mult))
                    vch(lambda v: v.tensor_tensor(out=C(11), in0=C(9), in1=C(3), op=ALU.mult))
                    vch(lambda v: v.tensor_tensor(out=tmp8, in0=ohg, in1=iota8, op=ALU.mult), [t_rt])
                    vch(lambda v: v.reduce_sum(out=C(12), in_=tmp8, axis=AX.X))
                    vch(lambda v: v.tensor_tensor(out=tmp8, in0=oh1, in1=iota8, op=ALU.mult))
                    vch(lambda v: v.reduce_sum(out=C(13), in_=tmp8, axis=AX.X))
                    vch(lambda v: v.tensor_tensor(out=tmp8, in0=oh2, in1=iota8, op=ALU.mult))
                    vch(lambda v: v.reduce_sum(out=C(14), in_=tmp8, axis=AX.X))
                    vch(lambda v: v.scalar_tensor_tensor(out=C(15), in0=C(12), scalar=8.0, in1=C(13), op0=ALU.mult, op1=ALU.add))
                    vch(lambda v: v.scalar_tensor_tensor(out=C(16), in0=C(12), scalar=8.0, in1=C(14), op0=ALU.mult, op1=ALU.add))
                    O1 = r64[:, 0, :]
                    O2 = r64[:, 1, :]
                    tm64 = r64[:, 2, :]
                    vch(lambda v: v.tensor_scalar(out=O1, in0=iota64, scalar1=C(15), scalar2=0.0, op0=ALU.is_equal, op1=ALU.add))
                    vch(lambda v: v.tensor_scalar(out=O2, in0=iota64, scalar1=C(16), scalar2=0.0, op0=ALU.is_equal, op1=ALU.add))
                    t_O = vch(lambda v, t=t: v.tensor_tensor(out=Oall[:, t, :], in0=O1, in1=O2, op=ALU.add))
                    O_tok.append(t_O)

                    def pre(pe, t=t):
                        inst = pe.matmul(out=banks[7][:, 0:64], lhsT=Umat, rhs=Oall[:, t, :], start=True, stop=(t == 0))
                        for tp in range(t):
                            inst = pe.matmul(out=banks[7][:, 0:64], lhsT=Ones, rhs=Oall[:, tp, :], start=False, stop=(tp == t - 1))
                        return inst
                    t_pre = PE(pre, [t_O, t_ut] + bank_last[7])
                    vch(lambda v: v.tensor_tensor(out=tm64, in0=banks[7][:, 0:64], in1=O1, op=ALU.mult), [t_pre])
                    vch(lambda v: v.reduce_sum(out=C(17), in_=tm64, axis=AX.X))
                    vch(lambda v: v.tensor_tensor(out=tm64, in0=banks[7][:, 0:64], in1=O2, op=ALU.mult))
                    t_p2 = vch(lambda v: v.reduce_sum(out=C(18), in_=tm64, axis=AX.X))
                    bank_last[7] = [t_p2]
                    for k in range(2):
                        vch(lambda v, k=k: v.tensor_scalar(out=C(19 + k), in0=C(17 + k), scalar1=float(CAP), scalar2=1.0,
                                                           op0=ALU.is_ge, op1=ALU.mult))
                    t_OV = vch(lambda v, t=t: v.tensor_copy(out=OVall[:, t, :], in_=rs[:, 19:21]))

                    def pre2(pe, t=t):
                        inst = pe.matmul(out=banks[7][:, 64:66], lhsT=Umat, rhs=OVall[:, t, :], start=True, stop=(t == 0))
                        for tp in range(t):
                            inst = pe.matmul(out=banks[7][:, 64:66], lhsT=Ones, rhs=OVall[:, tp, :], start=False,
                                             stop=(tp == t - 1))
                        return inst
                    t_pre2 = PE(pre2, [t_OV, t_p2])
                    t_c2 = vch(lambda v: v.tensor_copy(out=rs[:, 30:32], in_=banks[7][:, 64:66]), [t_pre2])
                    bank_last[7] = [t_c2]
                    vch(lambda v: v.tensor_tensor(out=C(24), in0=C(30), in1=C(31), op=ALU.add))
                    vch(lambda v: v.tensor_tensor(out=C(25), in0=C(24), in1=C(19), op=ALU.add))
                    for k in range(2):
                        vch(lambda v, k=k: v.scalar_tensor_tensor(out=C(21 + k), in0=C(15 + k), scalar=float(CAP), in1=C(17 + k),
                                                                  op0=ALU.mult, op1=ALU.add))
                        vch(lambda v, k=k: v.tensor_scalar(out=C(26 + k), in0=C(24 + k), scalar1=float(OVC), scalar2=1.0e6,
                                                           op0=ALU.is_ge, op1=ALU.mult))
                        vch(lambda v, k=k: v.tensor_tensor(out=C(26 + k), in0=C(26 + k), in1=C(24 + k), op=ALU.add))
                        vch(lambda v, k=k: v.scalar_tensor_tensor(out=C(26 + k), in0=C(26 + k), scalar=float(NEXP * CAP),
                                                                  in1=C(21 + k), op0=ALU.add, op1=ALU.subtract))
                        vch(lambda v, k=k: v.scalar_tensor_tensor(out=C(28 + k), in0=C(26 + k), scalar=C(19 + k),
                                                                  in1=C(21 + k), op0=ALU.mult, op1=ALU.add))
                    t_ix = vch(lambda v, b2=b2: v.tensor_copy(out=idx[b2][:], in_=rs[:, 28:30]), idx_rd[b2])
                    idx_rd[b2] = []
                    rows = (rowA[b2], rowB[b2])
                    metas = []
                    for k in range(2):
                        m1_ = vch(lambda v, k=k, rows=rows: v.tensor_copy(out=rows[k][:, D:D + 1], in_=C(10 + k)), [t_u2, t_cp])
                        m2_ = vch(lambda v, k=k, rows=rows, t=t: v.tensor_scalar(out=rows[k][:, D + 1:D + 2], in0=pid2,
                                                                            scalar1=float(2 * t * P + k + 1), scalar2=0.0,
                                                                            op0=ALU.add, op1=ALU.add))
                        m3_ = vch(lambda v, k=k, rows=rows: v.tensor_copy(out=rows[k][:, D + 2:D + 3], in_=C(15 + k)))
                        metas.append(m3_)
                    prev_route = ch[-1:]
                    for k in range(2):
                        t_s = S.dma("gpsimd", lambda g, k=k, b2=b2, rows=rows: g.indirect_dma_start(
                            out=xs[:, :], out_offset=bass.IndirectOffsetOnAxis(ap=idx[b2][:, k:k + 1], axis=0),
                            in_=rows[k][:, :], in_offset=None, bounds_check=getreg(g, NEXP * CAP + OVC - 1), oob_is_err=False),
                            sem_sc[2 * b2 + k], deps=[metas[1], t_ix, t_cp])
                        (rowA_rd if k == 0 else rowB_rd)[b2].append(t_s)
                        idx_rd[b2].append(t_s)
                S.barrier()
                bank_last = [[] for _ in range(8)]

                if "p3" not in dbg:
                    AR.off = phase_mark
                    wg = [AR.alloc(f"wg{i}", [P, KC, FF], BF16) for i in range(2)]
                    wu = [AR.alloc(f"wu{i}", [P, KC, FF], BF16) for i in range(2)]
                    wd = [AR.alloc(f"wd{i}", [P, 4, D], BF16) for i in range(2)]
                    xst = [AR.alloc(f"xst{i}", [P, RW], F32) for i in range(2)]
                    xb = AR.alloc("xb", [P, D], BF16)
                    xT = AR.alloc("xT", [P, KC, P], BF16)
                    sg = AR.alloc("sg", [P, FF], F32)
                    actb = AR.alloc("actb", [P, FF], BF16)
                    aT = AR.alloc("aT", [P, 4, P], BF16)
                    ys = [AR.alloc(f"ys{i}", [P, RW], F32) for i in range(2)]
                    idf = AR.alloc("idf", [P, 2], F32)
                    idi = [AR.alloc(f"idi{i}", [P, 2], I32) for i in range(2)]
                    sem_wg = [S.new_dma_sem() for _ in range(2)]
                    sem_wu = [S.new_dma_sem() for _ in range(2)]
                    sem_wd = [S.new_dma_sem() for _ in range(2)]
                    sem_xs = [S.new_dma_sem() for _ in range(2)]
                    sem_ys = [S.new_dma_sem() for _ in range(2)]
                    w_rd = [[], []]
                    xst_rd = [[], []]
                    ys_rd = [[], []]
                    xb_rd = []
                    xT_rd = []
                    aT_rd = []
                    act_rd = []
                    loads = {}

                    def load_expert(e):
                        b2 = e % 2
                        rd = w_rd[b2]
                        w_rd[b2] = []
                        tg = S.dma("gpsimd", lambda g: g.dma_start(out=wg[b2][:], in_=w_gate[e].rearrange("(c p) f -> p c f", p=P)),
                                   sem_wg[b2], deps=rd)
                        tu = S.dma("gpsimd", lambda g: g.dma_start(out=wu[b2][:], in_=w_up[e].rearrange("(c p) f -> p c f", p=P)),
                                   sem_wu[b2], deps=rd)
                        td = S.dma("gpsimd", lambda g: g.dma_start(out=wd[b2][:], in_=w_down[e].rearrange("(c p) d -> p c d", p=P)),
                                   sem_wd[b2], deps=rd)
                        tx = S.dma("sync", lambda q: q.dma_start(out=xst[b2][:], in_=xs[e * CAP:(e + 1) * CAP, :]), sem_xs[b2],
                                   deps=xst_rd[b2])
                        xst_rd[b2] = []
                        loads[e] = (tg, tu, td, tx)

                    xov = AR.alloc("xov", [P, RW], F32)
                    xovT = AR.alloc("xovT", [P, KC, P], BF16)
                    mw = AR.alloc("mw", [P, NEXP], F32)
                    acc = AR.alloc("acc", [P, RW], F32)
                    sg2 = AR.alloc("sg2", [P, FF], F32)
                    actb2 = AR.alloc("actb2", [P, FF], BF16)
                    aT2 = AR.alloc("aT2", [P, 4, P], BF16)
                    rt4 = AR.alloc("rt4", [P, 80], F32)
                    idv = AR.alloc("idv", [P, 2], I32)
                    sem_ov = [S.new_dma_sem() for _ in range(3)]
                    t_xov = S.dma("sync", lambda q: q.dma_start(out=xov[:], in_=xs[NEXP * CAP:NEXP * CAP + OVC, :]), sem_ov[0])
                    t_rt4 = S.dma("sync", lambda q: q.dma_start(out=rt4[:], in_=rtab_d), sem_ov[1])
                    t_xvb = VE(lambda v: v.tensor_copy(out=xb[:], in_=xov[:, 0:D]), [t_xov])
                    ovev = []
                    for half in range(2):
                        t_tr, t_ev = transpose8([xb[:, (half * 8 + k) * P:(half * 8 + k + 1) * P] for k in range(8)],
                                                xovT[:, half * 8:(half + 1) * 8, :], [t_xvb], [])
                        xb_rd.append(t_tr)
                        ovev.append(t_ev)
                    t_mw = VE(lambda v: v.tensor_scalar(out=mw[:], in0=rt4[:, 8:72], scalar1=xov[:, D + 2:D + 3],
                                                        scalar2=xov[:, D:D + 1], op0=ALU.is_equal, op1=ALU.mult),
                              [t_xov, t_rt4])
                    t_acc0 = VE(lambda v: v.memset(acc[:], 0.0))
                    acc_tok = [t_acc0, t_mw]
                    ov_rd = {"act": [], "aT": []}

                    load_expert(0)
                    for e in range(NEXP):
                        b2 = e % 2
                        if e + 1 < NEXP:
                            load_expert(e + 1)
                        tg, tu, td, tx = loads.pop(e)
                        t_xb = VE(lambda v, b2=b2: v.tensor_copy(out=xb[:], in_=xst[b2][:, 0:D]), [tx] + xb_rd)
                        xb_rd = []
                        evs = []
                        for half in range(2):
                            t_tr, t_ev = transpose8([xb[:, (half * 8 + k) * P:(half * 8 + k + 1) * P] for k in range(8)],
                                                    xT[:, half * 8:(half + 1) * 8, :], [t_xb], xT_rd)
                            xb_rd.append(t_tr)
                            evs.append(t_ev)
                        xT_rd = []

                        def gu(pe, b2=b2):
                            inst = None
                            for k in range(KC):
                                inst = pe.matmul(out=banks[2][:], lhsT=xT[:, k, :], rhs=wg[b2][:, k, :], start=(k == 0),
                                                 stop=(k == KC - 1))
                            for k in range(KC):
                                inst = pe.matmul(out=banks[3][:], lhsT=xT[:, k, :], rhs=wu[b2][:, k, :], start=(k == 0),
                                                 stop=(k == KC - 1))
                            return inst
                        t_gu = PE(gu, evs + [tg, tu] + bank_last[2] + bank_last[3])
                        xT_rd.append(t_gu)
                        t_sg = AC(lambda a: a.activation(out=sg[:], in_=banks[2][:], func=AF.Silu), [t_gu] + act_rd)
                        t_ac = VE(lambda v: v.tensor_tensor(out=actb[:], in0=banks[3][:], in1=sg[:], op=ALU.mult), [t_sg] + act_rd)
                        act_rd = []
                        bank_last[2] = [t_sg]
                        bank_last[3] = [t_ac]
                        bk = p01[0] % 2
                        p01[0] += 1
                        pb = banks[bk][:].bitcast(BF16)

                        def tra(pe, pb=pb):
                            inst = None
                            for k in range(4):
                                inst = pe.transpose(out=pb[:, k * P:(k + 1) * P], in_=actb[:, k * P:(k + 1) * P], identity=ident[:])
                            return inst
                        t_ta = PE(tra, [t_ac] + bank_last[bk])
                        act_rd.append(t_ta)
                        t_ea = AC(lambda a, pb=pb: a.copy(out=aT[:], in_=pb[:, 0:4 * P].rearrange("p (k t) -> p k t", k=4)),
                                  [t_ta] + aT_rd)
                        aT_rd = []
                        bank_last[bk] = [t_ea]
                        t_i0 = VE(lambda v, b2=b2: v.tensor_scalar(out=idf[:, 1:2], in0=xst[b2][:, D + 1:D + 2], scalar1=0.5,
                                                                   scalar2=1.0e6, op0=ALU.is_lt, op1=ALU.mult), [tx])
                        t_i1 = VE(lambda v, b2=b2: v.scalar_tensor_tensor(out=idf[:, 0:1], in0=xst[b2][:, D + 1:D + 2], scalar=-1.0,
                                                                          in1=idf[:, 1:2], op0=ALU.add, op1=ALU.add), [tx, t_i0])
                        t_i2 = VE(lambda v, b2=b2: v.tensor_copy(out=idi[b2][:], in_=idf[:, 0:2]), [t_i1] + ys_rd[b2])
                        dn = []
                        for nb in range(4):
                            def dm(pe, nb=nb, b2=b2):
                                inst = None
                                for k in range(4):
                                    inst = pe.matmul(out=banks[4 + nb][:], lhsT=aT[:, k, :], rhs=wd[b2][:, k, nb * 512:(nb + 1) * 512],
                                                     start=(k == 0), stop=(k == 3))
                                return inst
                            t_dm = PE(dm, [t_ea, td] + bank_last[4 + nb])
                            aT_rd.append(t_dm)
                            if nb % 2 == 0:
                                t_y = AC(lambda a, nb=nb, b2=b2: a.activation(out=ys[b2][:, nb * 512:(nb + 1) * 512],
                                                                              in_=banks[4 + nb][:], func=AF.Copy,
                                                                              scale=xst[b2][:, D:D + 1]), [t_dm] + ys_rd[b2])
                            else:
                                t_y = VE(lambda v, nb=nb, b2=b2: v.tensor_scalar_mul(out=ys[b2][:, nb * 512:(nb + 1) * 512],
                                                                                     in0=banks[4 + nb][:],
                                                                                     scalar1=xst[b2][:, D:D + 1]),
                                         [t_dm] + ys_rd[b2])
                            bank_last[4 + nb] = [t_y]
                            dn.append(t_y)
                        w_rd[b2] += [t_gu] + aT_rd[-4:]
                        t_sc = S.dma("gpsimd", lambda g, b2=b2: g.indirect_dma_start(
                            out=ypair[:, :], out_offset=bass.IndirectOffsetOnAxis(ap=idi[b2][:, 0:1], axis=0), in_=ys[b2][:, :],
                            in_offset=None, bounds_check=getreg(g, 2 * NQ * P - 1), oob_is_err=False), sem_ys[b2], deps=dn + [t_i2])
                        ys_rd[b2] = [t_sc]
                        xst_rd[b2] = dn + [t_xb, t_i1]
                        def gu2(pe, b2=b2):
                            inst = None
                            for k in range(KC):
                                inst = pe.matmul(out=banks[2][:], lhsT=xovT[:, k, :], rhs=wg[b2][:, k, :], start=(k == 0),
                                                 stop=(k == KC - 1))
                            for k in range(KC):
                                inst = pe.matmul(out=banks[3][:], lhsT=xovT[:, k, :], rhs=wu[b2][:, k, :], start=(k == 0),
                                                 stop=(k == KC - 1))
                            return inst
                        t_gu2 = PE(gu2, ovev + [tg, tu] + bank_last[2] + bank_last[3])
                        t_sg2 = AC(lambda a: a.activation(out=sg2[:], in_=banks[2][:], func=AF.Silu), [t_gu2] + ov_rd["act"])
                        t_ac2 = VE(lambda v: v.tensor_tensor(out=actb2[:], in0=banks[3][:], in1=sg2[:], op=ALU.mult),
                                   [t_sg2] + ov_rd["act"])
                        bank_last[2] = [t_sg2]
                        bank_last[3] = [t_ac2]
                        bk = p01[0] % 2
                        p01[0] += 1
                        pb = banks[bk][:].bitcast(BF16)

                        def tra2(pe, pb=pb):
                            inst = None
                            for k in range(4):
                                inst = pe.transpose(out=pb[:, k * P:(k + 1) * P], in_=actb2[:, k * P:(k + 1) * P],
                                                    identity=ident[:])
                            return inst
                        t_ta2 = PE(tra2, [t_ac2] + bank_last[bk])
                        ov_rd["act"] = [t_ta2]
                        t_ea2 = AC(lambda a, pb=pb: a.copy(out=aT2[:], in_=pb[:, 0:4 * P].rearrange("p (k t) -> p k t", k=4)),
                                   [t_ta2] + ov_rd["aT"])
                        ov_rd["aT"] = []
                        bank_last[bk] = [t_ea2]
                        new_acc = []
                        for nb in range(4):
                            def dm2(pe, nb=nb, b2=b2):
                                inst = None
                                for k in range(4):
                                    inst = pe.matmul(out=banks[4 + nb][:], lhsT=aT2[:, k, :],
                                                     rhs=wd[b2][:, k, nb * 512:(nb + 1) * 512], start=(k == 0), stop=(k == 3))
                                return inst
                            t_dm2 = PE(dm2, [t_ea2, td] + bank_last[4 + nb])
                            ov_rd["aT"].append(t_dm2)
                            t_a = VE(lambda v, nb=nb, e=e: v.scalar_tensor_tensor(
                                out=acc[:, nb * 512:(nb + 1) * 512], in0=banks[4 + nb][:], scalar=mw[:, e:e + 1],
                                in1=acc[:, nb * 512:(nb + 1) * 512], op0=ALU.mult, op1=ALU.add), [t_dm2] + acc_tok)
                            bank_last[4 + nb] = [t_a]
                            new_acc.append(t_a)
                        acc_tok = new_acc
                        w_rd[b2] += [t_gu2] + ov_rd["aT"]
                    t_v0 = VE(lambda v: v.tensor_scalar(out=idf[:, 1:2], in0=xov[:, D + 1:D + 2], scalar1=0.5, scalar2=1.0e6,
                                                        op0=ALU.is_lt, op1=ALU.mult), [t_xov, t_i2])
                    t_v1 = VE(lambda v: v.scalar_tensor_tensor(out=idf[:, 0:1], in0=xov[:, D + 1:D + 2], scalar=-1.0,
                                                               in1=idf[:, 1:2], op0=ALU.add, op1=ALU.add), [t_v0])
                    t_v2 = VE(lambda v: v.tensor_copy(out=idv[:], in_=idf[:, 0:2]), [t_v1])
                    S.dma("gpsimd", lambda g: g.indirect_dma_start(
                        out=ypair[:, :], out_offset=bass.IndirectOffsetOnAxis(ap=idv[:, 0:1], axis=0), in_=acc[:, :],
                        in_offset=None, bounds_check=getreg(g, 2 * NQ * P - 1), oob_is_err=False), sem_ov[2],
                        deps=acc_tok + [t_v2])
                    S.barrier()
                    bank_last = [[] for _ in range(8)]

                    AR.off = phase_mark
                    gfin = AR.alloc("gfin", [P, D], F32)
                    h5 = [AR.alloc(f"h5{i}", [P, D], F32) for i in range(2)]
                    y5 = [AR.alloc(f"y5{i}", [P, 2, RW], F32) for i in range(2)]
                    o5 = [AR.alloc(f"o5{i}", [P, D], F32) for i in range(2)]
                    junk5 = AR.alloc("junk5", [P, D], BF16)
                    ss5 = AR.alloc("ss5", [P, 32], F32)
                    sem_g5 = S.new_dma_sem()
                    t_g5 = S.dma("sync", lambda q: q.dma_start(out=gfin[:], in_=g_fin), sem_g5)
                    t_z5 = VE(lambda v: v.memset(ss5[:], 0.0))
                    sem_h5 = [S.new_dma_sem() for _ in range(2)]
                    sem_y5 = [S.new_dma_sem() for _ in range(2)]
                    sem_o5 = [S.new_dma_sem() for _ in range(2)]
                    h5_rd = [[], []]
                    y5_rd = [[], []]
                    o5_rd = [[], []]
                    for t in range(NQ):
                        b2 = t % 2
                        t_h = S.dma("sync", lambda q, t=t, b2=b2: q.dma_start(out=h5[b2][:], in_=h2d[t * P:(t + 1) * P, :]),
                                    sem_h5[b2], deps=h5_rd[b2])
                        t_y = S.dma("sync", lambda q, t=t, b2=b2: q.dma_start(
                            out=y5[b2][:], in_=ypair[2 * t * P:2 * (t + 1) * P, :].rearrange("(p k) d -> p k d", k=2)),
                            sem_y5[b2], deps=y5_rd[b2])
                        a1 = VE(lambda v, b2=b2: v.tensor_tensor(out=h5[b2][:], in0=h5[b2][:], in1=y5[b2][:, 0, 0:D], op=ALU.add),
                                [t_h, t_y])
                        a2 = VE(lambda v, b2=b2: v.tensor_tensor(out=h5[b2][:], in0=h5[b2][:], in1=y5[b2][:, 1, 0:D], op=ALU.add), [a1])
                        y5_rd[b2] = [a2]
                        t_sq = AC(lambda a, t=t, b2=b2: a.activation(out=junk5[:], in_=h5[b2][:], func=AF.Square,
                                                                    accum_out=ss5[:, t:t + 1]), [a2, t_z5])
                        t_r1 = AC(lambda a, t=t: a.activation(out=ss5[:, 16 + t:17 + t], in_=ss5[:, t:t + 1], func=AF.Sqrt,
                                                              bias=epst[:, 0:1], scale=1.0 / D), [t_sq])
                        t_r2 = VE(lambda v, t=t: v.reciprocal(out=ss5[:, 16 + t:17 + t], in_=ss5[:, 16 + t:17 + t]), [t_r1])
                        t_o = VE(lambda v, t=t, b2=b2: v.scalar_tensor_tensor(out=o5[b2][:], in0=h5[b2][:],
                                                                             scalar=ss5[:, 16 + t:17 + t], in1=gfin[:], op0=ALU.mult,
                                                                             op1=ALU.mult), [t_r2, t_g5] + o5_rd[b2])
                        h5_rd[b2] = [t_o, t_sq]
                        t_st = S.dma("scalar", lambda q, t=t, b2=b2: q.dma_start(out=out[t * P:(t + 1) * P, :], in_=o5[b2][:]),
                                     sem_o5[b2], deps=[t_o])
                        o5_rd[b2] = [t_st]

        S.barrier()

        dsems = [es.enter_context(nc.semaphore(f"ds{i}")) for i in range(len(S.dma_count))]
        with nc.Block() as block:
            S.emit(nc, block, esems, dsems)
    return nc


def _core_layout(x, meta_tokens, core):
    b, par = core // 2, core % 2
    n_pad = P - meta_tokens.shape[0]
    h0 = np.concatenate([np.zeros((n_pad, D), np.float32), meta_tokens.astype(np.float32), x[b]], axis=0)
    ch = h0.reshape(33, P, D)
    if par == 0:
        others = [None] + list(range(0, 31, 2))
        own = list(range(1, 32, 2))
    else:
        others = [0] + list(range(1, 32, 2))
        own = list(range(2, 33, 2))
    xo = np.stack([np.zeros((P, D), np.float32) if c is None else ch[c] for c in others]).reshape(NO * P, D)
    xq = np.stack([ch[c] for c in own]).reshape(NQ * P, D)
    return xo, xq, others, own


def _pos_tables(others, own, par):
    gam = np.array(GAM, np.float64)
    pidx = np.arange(P)
    inv_r = (1.0 / (10000.0 ** np.linspace(0.0, 1.0, 64, dtype=np.float32))).astype(np.float32)
    inv_d = (500000.0 ** (-np.arange(0, 32, 2, dtype=np.float32) / 32)).astype(np.float32)
    kdec = QSCALE * gam[None, :] ** (-(pidx[:, None] + 1.0))

    def chunk_tab(c, with_q):
        pos = (np.zeros(P) if c is None else c * P + pidx - 112).astype(np.float32)
        ang_r = (pos[:, None] * inv_r[None, :]).astype(np.float32).astype(np.float64)
        cr, sr = np.cos(ang_r), np.sin(ang_r)
        ck = (cr[:, None, :] * kdec[:, :, None]).reshape(P, 256)
        sk = (sr[:, None, :] * kdec[:, :, None]).reshape(P, 256)
        ang_d = (pos[:, None] * inv_d[None, :]).astype(np.float32).astype(np.float64)
        cd_, sd_ = np.cos(ang_d), np.sin(ang_d)
        parts = [ck, sk]
        if with_q:
            parts += [np.tile(cr, (1, 4)), np.tile(sr, (1, 4))]
        parts += [np.tile(cd_, (1, 8)), np.tile(sd_, (1, 8))]
        return np.concatenate(parts, 1).astype(np.float32)

    tab_o = np.stack([chunk_tab(c, False) for c in others])
    tab_q = np.stack([chunk_tab(c, True) for c in own])
    tri = (pidx[:, None] <= pidx[None, :]).astype(np.float32)
    tri4 = np.tile(tri, (1, 4))
    xi = (gam[None, :] ** (pidx[:, None] + 1.0)).astype(np.float32)
    m_tri = np.where(pidx[:, None] <= pidx[None, :], 0.0, NEG).astype(np.float32)
    m_pad = np.where(pidx[:, None] >= 112, 0.0, NEG).astype(np.float32) * np.ones((1, P), np.float32)
    m_all = np.full((P, P), NEG, np.float32)
    m_none = np.zeros((P, P), np.float32)
    if par == 0:
        mt = [m_all, m_pad, m_tri]
    else:
        mt = [m_pad, m_none, m_tri]
    mtab = np.concatenate(mt, 1).astype(ml_dtypes.bfloat16)
    return tab_o, tab_q, tri4, xi, mtab


def _bc(v):
    return np.ascontiguousarray(np.broadcast_to(np.asarray(v, np.float32).reshape(1, -1), (P, v.size)))


def kernel(**inputs):
    dbg = DEBUG
    x = np.asarray(inputs["x"], np.float32)
    meta = np.asarray(inputs["meta_tokens"], np.float32)
    nc = build_program(dbg)
    in_maps = []
    lamv = np.concatenate([inputs["lambda_q1"][0], inputs["lambda_k1"][0], inputs["lambda_q2"][0],
                           inputs["lambda_k2"][0]]).astype(np.float32)
    wgate = np.ascontiguousarray(inputs["w_gate"][0], np.float32)
    wup = np.ascontiguousarray(inputs["w_up"][0], np.float32)
    wdown = np.ascontiguousarray(inputs["w_down"][0], np.float32)
    pidx = np.arange(P)
    rtab = np.zeros((P, 80), np.float32)
    rtab[:, 0:8] = np.arange(8)[None, :]
    rtab[:, 8:72] = np.arange(64)[None, :]
    rtab[:, 72] = 2 * pidx
    utab = np.concatenate([(pidx[:, None] < pidx[None, :]).astype(np.float32), np.ones((P, P), np.float32)],
                          1).astype(ml_dtypes.bfloat16)
    for core in range(8):
        xo, xq, others, own = _core_layout(x, meta, core)
        tab_o, tab_q, tri4, xi, mtab = _pos_tables(others, own, core % 2)
        ctab = np.concatenate([tri4, xi, _bc(lamv), _bc(inputs["diff_subln_g"][0])], 1).astype(np.float32)
        m = {
            "xo": xo, "xq": xq,
            "w_in": np.ascontiguousarray(inputs["w_in"][0], np.float32),
            "g_attn": _bc(inputs["attn_norm_g"][0]),
            "tab_o": tab_o, "tab_q": tab_q, "ctab": np.ascontiguousarray(ctab), "mtab": mtab,
            "w_out": np.ascontiguousarray(inputs["w_out"][0], np.float32),
            "wr": np.ascontiguousarray(np.concatenate([inputs["w_group_router"][0], inputs["w_expert_router"][0]], 1),
                                       np.float32),
            "g_ffn": _bc(inputs["ffn_norm_g"][0]), "g_fin": _bc(inputs["final_norm_g"]),
            "rtab": rtab, "utab": utab,
        }
        if dbg in ("", "full"):
            m["w_gate"] = wgate
            m["w_up"] = wup
            m["w_down"] = wdown
        in_maps.append(m)
    res = run_bass_kernel_spmd(nc, in_maps, core_ids=list(range(8)))
    if dbg:
        return res
    outs = [r["out"] for r in res.results]
    full = np.zeros((4, 4096, D), np.float32)
    for core in range(8):
        b, par = core // 2, core % 2
        o = outs[core].reshape(NQ, P, D)
        for j in range(NQ):
            c = 2 * j + 1 + par
            full[b, (c - 1) * P:c * P, :] = o[j]
    return full
```
